# Optimizing a Trainium2 kernel written in Bass

```python
import jax
import jax.numpy as jnp
from jax import lax
import numpy as np

D_MODEL = 1024
BATCH = 16
SEQ = 2048
DEPTH = 2

HEAD_DIM = 64
BLOCK_Q = 128
A_HEADS = 8
A_KV_HEADS = 2
A_WINDOW = 128
B_HEADS = 8
B_KV_HEADS = 2
CMP_LEN = 32
CMP_STRIDE = 16
CMP_HIDDEN = 256
SLC_BLOCK = 64
SLC_TOPN = 8
B_WINDOW = 256
N_ATT_HEADS = A_HEADS + B_HEADS
A_Q_W = A_HEADS * HEAD_DIM
A_KV_W = A_KV_HEADS * HEAD_DIM
B_Q_W = B_HEADS * HEAD_DIM
B_KV_W = B_KV_HEADS * HEAD_DIM
N_GATES = 3 * B_HEADS
IN_SPLITS = (A_Q_W, A_KV_W, A_KV_W, B_Q_W, B_KV_W, B_KV_W, B_KV_W, B_KV_W, B_KV_W, B_KV_W, N_GATES)
IN_PROJ_W = sum(IN_SPLITS)
ATT_W = N_ATT_HEADS * HEAD_DIM
GMLP_W = D_MODEL
GMLP_GROUPS = 8
GMLP_CHUNK = 128
D_FF = 2816
N_EXPERTS = 8
TOP_K = 2
D_FF_EXPERT = 3584
MOE_BLOCK = 256
DN_ALPHA = (2.0 * DEPTH) ** 0.25
DN_BETA = (8.0 * DEPTH) ** -0.25
LN_EPS = 1e-5
N_EVEN = (DEPTH + 1) // 2
N_ODD = DEPTH // 2

kernel_name = 'hybrid_swa_nsa_gmlp_moe_deepnorm'


def layer_norm(x, g, b):
    xf = x.astype(jnp.float32)
    mu = jnp.mean(xf, axis=-1, keepdims=True)
    var = jnp.mean(jnp.square(xf - mu), axis=-1, keepdims=True)
    return ((xf - mu) * lax.rsqrt(var + LN_EPS)).astype(x.dtype) * g + b


def masked_softmax(s, mask, sink=None):
    s = jnp.where(mask, s, -jnp.inf)
    m = jnp.max(s, axis=-1, keepdims=True)
    if sink is not None:
        m = jnp.maximum(m, sink)
    m = jnp.where(jnp.isfinite(m), m, 0.0)
    e = jnp.where(mask, jnp.exp(s - m), 0.0)
    den = jnp.sum(e, axis=-1, keepdims=True)
    if sink is not None:
        den = den + jnp.exp(sink - m)
    return e / jnp.where(den > 0.0, den, 1.0)


def alibi_slopes():
    return 2.0 ** (-8.0 * jnp.arange(1, N_ATT_HEADS + 1, dtype=jnp.float32) / N_ATT_HEADS)


def banded_gqa(q, k, v, slopes, window, sink=None):
    bsz, seq, hkv, grp, hd = q.shape
    nblk = seq // BLOCK_Q
    n_prev = -(-window // BLOCK_Q)
    span = (n_prev + 1) * BLOCK_Q
    pad = ((0, 0), (n_prev * BLOCK_Q, 0), (0, 0), (0, 0))
    kb = jnp.pad(k, pad).reshape(bsz, nblk + n_prev, BLOCK_Q, hkv, hd)
    vb = jnp.pad(v, pad).reshape(bsz, nblk + n_prev, BLOCK_Q, hkv, hd)
    k_band = jnp.concatenate([kb[:, j:j + nblk] for j in range(n_prev + 1)], axis=2)
    v_band = jnp.concatenate([vb[:, j:j + nblk] for j in range(n_prev + 1)], axis=2)
    qb = q.reshape(bsz, nblk, BLOCK_Q, hkv, grp, hd)
    s = jnp.einsum('bnqhgd,bnkhd->bnhgqk', qb, k_band).astype(jnp.float32) * (hd ** -0.5)
    q_pos = jnp.arange(nblk)[:, None] * BLOCK_Q + jnp.arange(BLOCK_Q)[None, :]
    k_pos = jnp.arange(nblk)[:, None] * BLOCK_Q - n_prev * BLOCK_Q + jnp.arange(span)[None, :]
    dist = q_pos[:, :, None] - k_pos[:, None, :]
    mask = (dist >= 0) & (dist < window) & (k_pos[:, None, :] >= 0)
    s = s - slopes[None, None, :, :, None, None] * dist[None, :, None, None].astype(jnp.float32)
    sink_b = None if sink is None else sink.astype(jnp.float32)[None, None, :, :, None, None]
    p = masked_softmax(s, mask[None, :, None, None], sink_b)
    o = jnp.einsum('bnhgqk,bnkhd->bnqhgd', p.astype(v.dtype), v_band)
    return o.reshape(bsz, seq, hkv, grp, hd)


def nsa_compress(k, pe, w1, w2):
    bsz, seq, hkv, hd = k.shape
    r = CMP_LEN // CMP_STRIDE
    n_str = seq // CMP_STRIDE
    n_cmp = n_str - r + 1
    ks = k.reshape(bsz, n_str, CMP_STRIDE, hkv, hd)
    win = jnp.concatenate([ks[:, j:j + n_cmp] for j in range(r)], axis=2)
    win = win + pe[None, None, :, None, :]
    flat = jnp.moveaxis(win, 3, 2).reshape(bsz, n_cmp, hkv, CMP_LEN * hd)
    return jax.nn.gelu(flat @ w1) @ w2


def nsa_attention(q, k_cmp, v_cmp, k_slc, v_slc, k_win, v_win, gates, slopes, pe_k, wk1, wk2, pe_v, wv1, wv2):
    bsz, seq, hkv, grp, hd = q.shape
    scale = hd ** -0.5
    t = jnp.arange(seq)
    kc = nsa_compress(k_cmp, pe_k, wk1, wk2)
    vc = nsa_compress(v_cmp, pe_v, wv1, wv2)
    n_cmp = kc.shape[1]
    c_start = jnp.arange(n_cmp) * CMP_STRIDE
    c_end = c_start + CMP_LEN - 1
    dist_c = t[:, None] - c_end[None, :]
    s_c = jnp.einsum('bthgd,bchd->bhgtc', q, kc).astype(jnp.float32) * scale
    s_c = s_c - slopes[None, :, :, None, None] * dist_c.astype(jnp.float32)
    p_c = masked_softmax(s_c, (dist_c >= 0)[None, None, None])
    o_cmp = jnp.einsum('bhgtc,bchd->bthgd', p_c.astype(vc.dtype), vc)
    n_sel = seq // SLC_BLOCK
    s_start = jnp.arange(n_sel) * SLC_BLOCK
    overlap = ((c_start[:, None] < s_start[None, :] + SLC_BLOCK) & (c_start[:, None] + CMP_LEN > s_start[None, :])).astype(jnp.float32)
    imp = jnp.einsum('bhgtc,cj->bhtj', p_c, overlap)
    t_blk = t // SLC_BLOCK
    jb = jnp.arange(n_sel)
    valid = jb[None, :] <= t_blk[:, None]
    forced = (jb[None, :] == 0) | (jb[None, :] == t_blk[:, None]) | (jb[None, :] == t_blk[:, None] - 1)
    rank = jnp.where(forced, jnp.inf, jnp.where(valid, imp, -jnp.inf))
    top_n = min(SLC_TOPN, n_sel)
    _, sel_idx = lax.top_k(rank, top_n)
    nq = seq // BLOCK_Q
    kb = k_slc.reshape(bsz, n_sel, SLC_BLOCK, hkv, hd).transpose(0, 3, 1, 2, 4)
    vb = v_slc.reshape(bsz, n_sel, SLC_BLOCK, hkv, hd).transpose(0, 3, 1, 2, 4)
    q_blocks = jnp.moveaxis(q.reshape(bsz, nq, BLOCK_Q, hkv, grp, hd), 1, 0)
    i_blocks = jnp.moveaxis(sel_idx.reshape(bsz, hkv, nq, BLOCK_Q, top_n), 2, 0)
    b_ix = jnp.arange(bsz)[:, None, None, None]
    h_ix = jnp.arange(hkv)[None, :, None, None]
    span = top_n * SLC_BLOCK

    def selected_block(args):
        qi, ii, t0 = args
        kg = kb[b_ix, h_ix, ii].reshape(bsz, hkv, BLOCK_Q, span, hd)
        vg = vb[b_ix, h_ix, ii].reshape(bsz, hkv, BLOCK_Q, span, hd)
        s = jnp.einsum('bqhgd,bhqmd->bhgqm', qi, kg).astype(jnp.float32) * scale
        k_pos = (ii[..., None] * SLC_BLOCK + jnp.arange(SLC_BLOCK)).reshape(bsz, hkv, BLOCK_Q, span)
        dist = (t0 + jnp.arange(BLOCK_Q))[None, None, :, None] - k_pos
        s = s - slopes[None, :, :, None, None] * dist[:, :, None].astype(jnp.float32)
        p = masked_softmax(s, (dist >= 0)[:, :, None])
        return jnp.einsum('bhgqm,bhqmd->bqhgd', p.astype(vg.dtype), vg)

    o_slc = lax.map(selected_block, (q_blocks, i_blocks, jnp.arange(nq) * BLOCK_Q))
    o_slc = jnp.moveaxis(o_slc, 0, 1).reshape(bsz, seq, hkv, grp, hd)
    o_win = banded_gqa(q, k_win, v_win, slopes, B_WINDOW)
    g = jax.nn.sigmoid(gates.astype(jnp.float32)).astype(q.dtype).reshape(bsz, seq, hkv, grp, 3)
    return g[..., 0:1] * o_cmp + g[..., 1:2] * o_slc + g[..., 2:3] * o_win


def hybrid_attention(h, w_in, sinks, pe_k, wk1, wk2, pe_v, wv1, wv2, w_o):
    bsz, seq, _ = h.shape
    proj = h @ w_in
    offs = np.cumsum(IN_SPLITS)[:-1].tolist()
    qa, ka, va, qb, kc, vc, ks, vs, kw, vw, gates = jnp.split(proj, offs, axis=-1)
    ga = A_HEADS // A_KV_HEADS
    gb = B_HEADS // B_KV_HEADS
    slopes = alibi_slopes()

    def kv_heads(z, n):
        return z.reshape(bsz, seq, n, HEAD_DIM)

    o_a = banded_gqa(qa.reshape(bsz, seq, A_KV_HEADS, ga, HEAD_DIM), kv_heads(ka, A_KV_HEADS), kv_heads(va, A_KV_HEADS),
                     slopes[:A_HEADS].reshape(A_KV_HEADS, ga), A_WINDOW, sinks.reshape(A_KV_HEADS, ga))
    o_b = nsa_attention(qb.reshape(bsz, seq, B_KV_HEADS, gb, HEAD_DIM),
                        kv_heads(kc, B_KV_HEADS), kv_heads(vc, B_KV_HEADS),
                        kv_heads(ks, B_KV_HEADS), kv_heads(vs, B_KV_HEADS),
                        kv_heads(kw, B_KV_HEADS), kv_heads(vw, B_KV_HEADS),
                        gates, slopes[A_HEADS:].reshape(B_KV_HEADS, gb), pe_k, wk1, wk2, pe_v, wv1, wv2)
    o = jnp.concatenate([o_a.reshape(bsz, seq, A_Q_W), o_b.reshape(bsz, seq, B_Q_W)], axis=-1)
    return o @ w_o


def chunked_gmlp(h, w_in, ln_g, ln_b, w_s, b_s, w_out):
    bsz, seq, _ = h.shape
    u, v = jnp.split(jax.nn.gelu(h @ w_in), 2, axis=-1)
    v = layer_norm(v, ln_g, ln_b)
    gd = GMLP_W // GMLP_GROUPS
    vc = v.reshape(bsz, seq // GMLP_CHUNK, GMLP_CHUNK, GMLP_GROUPS, gd)
    mixed = jnp.einsum('gts,bcsgd->bctgd', jnp.tril(w_s), vc) + b_s.T[None, None, :, :, None]
    return (u * mixed.reshape(bsz, seq, GMLP_W)) @ w_out


def swiglu(h, w_gate, w_up, w_down):
    return (jax.nn.silu(h @ w_gate) * (h @ w_up)) @ w_down


def moe_swiglu(h, w_router, w_gate, w_up, w_down):
    bsz, seq, dm = h.shape
    xf = h.reshape(-1, dm)
    n_tok = xf.shape[0]
    logits = (xf @ w_router).astype(jnp.float32)
    top_val, top_idx = lax.top_k(logits, TOP_K)
    gate = jax.nn.softmax(top_val, axis=-1)
    n_asg = n_tok * TOP_K
    flat_e = top_idx.reshape(-1)
    flat_tok = jnp.repeat(jnp.arange(n_tok, dtype=jnp.int32), TOP_K)
    flat_g = gate.reshape(-1)
    order = jnp.argsort(flat_e)
    e_sorted = flat_e[order]
    tok_sorted = flat_tok[order]
    g_sorted = flat_g[order]
    counts = jnp.bincount(flat_e, length=N_EXPERTS)
    starts = jnp.cumsum(counts) - counts
    padded = (counts + MOE_BLOCK - 1) // MOE_BLOCK * MOE_BLOCK
    pad_ends = jnp.cumsum(padded)
    pad_starts = pad_ends - padded
    dest = pad_starts[e_sorted] + (jnp.arange(n_asg) - starts[e_sorted])
    n_blocks = -(-n_asg // MOE_BLOCK) + N_EXPERTS
    n_rows = n_blocks * MOE_BLOCK
    row_tok = jnp.full((n_rows,), n_tok, dtype=jnp.int32).at[dest].set(tok_sorted)
    x_pad = jnp.concatenate([xf, jnp.zeros((1, dm), xf.dtype)], axis=0)
    xin = x_pad[row_tok].reshape(n_blocks, MOE_BLOCK, dm)
    blk_e = jnp.minimum(jnp.searchsorted(pad_ends, jnp.arange(n_blocks) * MOE_BLOCK, side='right'), N_EXPERTS - 1)

    def expert_block(args):
        xb, e = args
        return (jax.nn.silu(xb @ w_gate[e]) * (xb @ w_up[e])) @ w_down[e]

    yb = lax.map(expert_block, (xin, blk_e)).reshape(n_rows, dm)
    contrib = yb[dest] * g_sorted[:, None].astype(yb.dtype)
    y = jnp.zeros((n_tok, dm), yb.dtype).at[tok_sorted].add(contrib)
    return y.reshape(bsz, seq, dm)


def setup_inputs(seed: int = 0) -> dict:
    key = jax.random.key(seed)
    ks = jax.random.split(key, 26)
    f32 = jnp.float32

    def nrm(k, shape, scale):
        return jax.random.normal(k, shape, f32) * scale

    dm = D_MODEL
    cmp_in = CMP_LEN * HEAD_DIM
    return {
        'x': nrm(ks[0], (BATCH, SEQ, dm), 1.0),
        'att_w_in': nrm(ks[1], (N_EVEN, dm, IN_PROJ_W), dm ** -0.5),
        'att_sinks': nrm(ks[2], (N_EVEN, A_HEADS), 0.5),
        'cmp_pe_k': nrm(ks[3], (N_EVEN, CMP_LEN, HEAD_DIM), 0.1),
        'cmp_wk1': nrm(ks[4], (N_EVEN, cmp_in, CMP_HIDDEN), cmp_in ** -0.5),
        'cmp_wk2': nrm(ks[5], (N_EVEN, CMP_HIDDEN, HEAD_DIM), CMP_HIDDEN ** -0.5),
        'cmp_pe_v': nrm(ks[6], (N_EVEN, CMP_LEN, HEAD_DIM), 0.1),
        'cmp_wv1': nrm(ks[7], (N_EVEN, cmp_in, CMP_HIDDEN), cmp_in ** -0.5),
        'cmp_wv2': nrm(ks[8], (N_EVEN, CMP_HIDDEN, HEAD_DIM), CMP_HIDDEN ** -0.5),
        'att_w_o': nrm(ks[9], (N_EVEN, ATT_W, dm), DN_BETA * ATT_W ** -0.5),
        'ffn_w_gate': nrm(ks[10], (N_EVEN, dm, D_FF), dm ** -0.5),
        'ffn_w_up': nrm(ks[11], (N_EVEN, dm, D_FF), dm ** -0.5),
        'ffn_w_down': nrm(ks[12], (N_EVEN, D_FF, dm), DN_BETA * D_FF ** -0.5),
        'gmlp_w_in': nrm(ks[13], (N_ODD, dm, 2 * GMLP_W), dm ** -0.5),
        'gmlp_ln_g': 1.0 + nrm(ks[14], (N_ODD, GMLP_W), 0.02),
        'gmlp_ln_b': nrm(ks[15], (N_ODD, GMLP_W), 0.02),
        'gmlp_w_s': nrm(ks[16], (N_ODD, GMLP_GROUPS, GMLP_CHUNK, GMLP_CHUNK), GMLP_CHUNK ** -0.5),
        'gmlp_b_s': 1.0 + nrm(ks[17], (N_ODD, GMLP_GROUPS, GMLP_CHUNK), 0.1),
        'gmlp_w_out': nrm(ks[18], (N_ODD, GMLP_W, dm), DN_BETA * GMLP_W ** -0.5),
        'moe_w_router': nrm(ks[19], (N_ODD, dm, N_EXPERTS), dm ** -0.5),
        'moe_w_gate': nrm(ks[20], (N_ODD, N_EXPERTS, dm, D_FF_EXPERT), dm ** -0.5),
        'moe_w_up': nrm(ks[21], (N_ODD, N_EXPERTS, dm, D_FF_EXPERT), dm ** -0.5),
        'moe_w_down': nrm(ks[22], (N_ODD, N_EXPERTS, D_FF_EXPERT, dm), DN_BETA * D_FF_EXPERT ** -0.5),
        'ln_g': 1.0 + nrm(ks[23], (DEPTH, 2, dm), 0.02),
        'ln_b': nrm(ks[24], (DEPTH, 2, dm), 0.02),
    }


def reference(x, att_w_in, att_sinks, cmp_pe_k, cmp_wk1, cmp_wk2, cmp_pe_v, cmp_wv1, cmp_wv2, att_w_o,
              ffn_w_gate, ffn_w_up, ffn_w_down, gmlp_w_in, gmlp_ln_g, gmlp_ln_b, gmlp_w_s, gmlp_b_s, gmlp_w_out,
              moe_w_router, moe_w_gate, moe_w_up, moe_w_down, ln_g, ln_b):
    for i in range(DEPTH):
        j = i // 2
        if i % 2 == 0:
            mix = hybrid_attention(x, att_w_in[j], att_sinks[j], cmp_pe_k[j], cmp_wk1[j], cmp_wk2[j],
                                   cmp_pe_v[j], cmp_wv1[j], cmp_wv2[j], att_w_o[j])
            x = layer_norm(DN_ALPHA * x + mix, ln_g[i, 0], ln_b[i, 0])
            ff = swiglu(x, ffn_w_gate[j], ffn_w_up[j], ffn_w_down[j])
        else:
            mix = chunked_gmlp(x, gmlp_w_in[j], gmlp_ln_g[j], gmlp_ln_b[j], gmlp_w_s[j], gmlp_b_s[j], gmlp_w_out[j])
            x = layer_norm(DN_ALPHA * x + mix, ln_g[i, 0], ln_b[i, 0])
            ff = moe_swiglu(x, moe_w_router[j], moe_w_gate[j], moe_w_up[j], moe_w_down[j])
        x = layer_norm(DN_ALPHA * x + ff, ln_g[i, 1], ln_b[i, 1])
    return x
```

```python
import numpy as np
from contextlib import ExitStack
import ml_dtypes
import concourse.bass as bass
import concourse.mybir as mybir
from concourse.bass_utils import run_bass_kernel_spmd

F32 = mybir.dt.float32
BF16 = mybir.dt.bfloat16
AF = mybir.ActivationFunctionType
ALU = mybir.AluOpType
AX = mybir.AxisListType

ENGS = ("pe", "act", "dve", "pool", "sp")
DMAQ = ("sp", "pool")
NDMA = 8

NTOK = 4096
SEQ = 2048
DM = 1024
ALPHA = 4.0 ** 0.25
LN_EPS = 1e-5
SLOPES = [2.0 ** (-8.0 * h / 16.0) for h in range(1, 17)]
MASKV = 1.0e5
D_FF = 2816
D_FFE = 3584
NEXP = 8


class Buf:
    __slots__ = ("name", "w", "r")

    def __init__(self, name=None):
        self.name = name
        self.w = None
        self.r = []


class Ins:
    __slots__ = ("fn", "waits", "inc", "dma")

    def __init__(self, fn):
        self.fn = fn
        self.waits = []
        self.inc = False
        self.dma = None


class Glob:
    def __init__(self, nc, stack):
        self.nc = nc
        self.esem = {e: stack.enter_context(nc.semaphore("e_" + e)) for e in ENGS}
        self.ecnt = {e: 0 for e in ENGS}
        self.dsem = {}
        self.dcnt = {}
        for q in DMAQ:
            for k in range(NDMA):
                self.dsem[(q, k)] = stack.enter_context(nc.semaphore("d_%s%d" % (q, k)))
                self.dcnt[(q, k)] = 0
        self.drr = {q: 0 for q in DMAQ}


class Phase:
    def __init__(self, g, name):
        self.g = g
        self.nc = g.nc
        self.name = name
        self.stack = ExitStack()
        self.ins = {e: [] for e in ENGS}
        self.seen = {e: {} for e in ENGS}
        g.nphase = getattr(g, "nphase", 0) + 1
        self.pid = g.nphase

    def sb(self, name, shape, dt):
        return self.stack.enter_context(self.nc.sbuf_tensor("%s_%s" % (self.name, name), list(shape), dt))

    def ps(self, name, shape, dt):
        return self.stack.enter_context(self.nc.psum_tensor("%s_%s" % (self.name, name), list(shape), dt))

    def _deps(self, eng, I, reads, writes):
        raw = set()
        oth = set()
        for b in reads:
            if b.w is not None:
                raw.add(b.w)
        for b in writes:
            if b.w is not None:
                oth.add(b.w)
            for t in b.r:
                oth.add(t)
        me = self.pid
        for t in raw | oth:
            if t[-1] != me:
                continue
            if t[0] == "e":
                if t[1] == eng:
                    if eng == "pe":
                        continue
                key = ("e", t[1])
                val = t[2]
            else:
                key = ("d", t[1], t[2])
                val = t[3]
            if self.seen[eng].get(key, -1) >= val:
                continue
            self.seen[eng][key] = val
            I.waits.append((key, val))
            if t[0] == "e":
                self.ins[t[1]][t[2]].inc = True

    def op(self, eng, fn, reads=(), writes=()):
        I = Ins(fn)
        self._deps(eng, I, reads, writes)
        idx = len(self.ins[eng])
        self.ins[eng].append(I)
        tok = ("e", eng, idx, self.pid)
        for b in reads:
            b.r.append(tok)
        for b in writes:
            b.w = tok
            b.r = []
        return tok

    def dma(self, q, out, in_, reads=(), writes=()):
        g = self.g
        k = g.drr[q]
        g.drr[q] = (k + 1) % NDMA
        I = Ins(lambda e, o=out, i=in_: e.dma_start(out=o, in_=i))
        I.dma = (q, k)
        self._deps(q, I, reads, writes)
        prev = g.dcnt[(q, k)]
        key = ("d", q, k)
        if prev > 0 and self.seen[q].get(key, -1) < prev:
            self.seen[q][key] = prev
            I.waits.append((key, prev))
        g.dcnt[(q, k)] = prev + 16
        self.ins[q].append(I)
        tok = ("d", q, k, prev + 16, self.pid)
        for b in reads:
            b.r.append(tok)
        for b in writes:
            b.w = tok
            b.r = []
        return tok

    def finish(self):
        g = self.g
        nc = self.nc
        last = {}
        for e in ENGS:
            for idx in range(len(self.ins[e]) - 1, -1, -1):
                if self.ins[e][idx].dma is None and self.ins[e][idx].fn is not None:
                    last[e] = idx
                    self.ins[e][idx].inc = True
                    break
        for e in ENGS:
            I = Ins(None)
            for x in ENGS:
                if x != e and x in last:
                    if self.seen[e].get(("e", x), -1) < last[x]:
                        I.waits.append((("e", x), last[x]))
            for (q, k), v in g.dcnt.items():
                if v > 0 and self.seen[e].get(("d", q, k), -1) < v:
                    I.waits.append((("d", q, k), v))
            self.ins[e].append(I)
        pref = {}
        for e in ENGS:
            c = g.ecnt[e]
            arr = []
            for I in self.ins[e]:
                if I.inc and I.dma is None and I.fn is not None:
                    c += 1
                arr.append(c)
            pref[e] = arr
            g.ecnt[e] = c

        def resolve(key, val):
            if key[0] == "e":
                return g.esem[key[1]], pref[key[1]][val]
            return g.dsem[(key[1], key[2])], val

        def mk(ename):
            def body(e):
                for I in self.ins[ename]:
                    for key, val in I.waits:
                        s, v = resolve(key, val)
                        e.wait_ge(s, v)
                    if I.fn is not None:
                        bi = I.fn(e)
                        if I.dma is not None:
                            bi.then_inc(g.dsem[I.dma], 16)
                        elif I.inc:
                            bi.then_inc(g.esem[ename], 1)
            return body

        with nc.Block() as block:
            block.tensor(mk("pe"))
            block.scalar(mk("act"))
            block.vector(mk("dve"))
            block.gpsimd(mk("pool"))
            block.sync(mk("sp"))
        self.stack.close()

    def mm(self, out, lhsT, rhs, start, stop, reads, writes):
        self.op("pe", lambda e, o=out, l=lhsT, r=rhs, s=start, t=stop: e.matmul(o, l, r, start=s, stop=t), reads, writes)

    def tr(self, out, in_, ident, reads, writes):
        self.op("pe", lambda e, o=out, i=in_, d=ident: e.transpose(out=o, in_=i, identity=d), reads, writes)

    def act(self, out, in_, func, reads, writes, bias=None, scale=1.0, accum=None):
        def fn(e, o=out, i=in_, f=func, b=bias, s=scale, a=accum):
            kw = {}
            if b is not None:
                kw["bias"] = b
            if a is not None:
                kw["accum_out"] = a
            return e.activation(out=o, in_=i, func=f, scale=s, **kw)
        self.op("act", fn, reads, writes)

    def ts(self, eng, out, in0, s1, s2, op0, op1, reads, writes):
        def fn(e, o=out, i=in0, a=s1, b=s2, p=op0, q=op1):
            if q is None:
                return e.tensor_scalar(out=o, in0=i, scalar1=a, scalar2=None, op0=p)
            return e.tensor_scalar(out=o, in0=i, scalar1=a, scalar2=b, op0=p, op1=q)
        self.op(eng, fn, reads, writes)

    def stt(self, eng, out, in0, scalar, in1, op0, op1, reads, writes):
        self.op(eng, lambda e, o=out, i=in0, s=scalar, j=in1, p=op0, q=op1: e.scalar_tensor_tensor(out=o, in0=i, scalar=s, in1=j, op0=p, op1=q), reads, writes)

    def tt(self, eng, out, in0, in1, op, reads, writes):
        self.op(eng, lambda e, o=out, i=in0, j=in1, p=op: e.tensor_tensor(out=o, in0=i, in1=j, op=p), reads, writes)

    def cp(self, eng, out, in_, reads, writes):
        if eng == "act":
            self.op("act", lambda e, o=out, i=in_: e.copy(out=o, in_=i), reads, writes)
        else:
            self.op(eng, lambda e, o=out, i=in_: e.tensor_copy(out=o, in_=i), reads, writes)

    def memset(self, eng, ap, val, writes):
        self.op(eng, lambda e, a=ap, v=val: e.memset(a, v), (), writes)

    def rmax(self, out, in_, reads, writes):
        self.op("dve", lambda e, o=out, i=in_: e.reduce_max(out=o, in_=i, axis=AX.X), reads, writes)

    def recip(self, out, in_, reads, writes):
        self.op("dve", lambda e, o=out, i=in_: e.reciprocal(out=o, in_=i), reads, writes)


class LNCtx:
    def __init__(self, ph, g_row, b_row, tag="ln", nslot=2):
        self.ph = ph
        self.ns = nslot
        self.gam = ph.sb(tag + "_g", [128, DM], F32)
        self.bet = ph.sb(tag + "_b", [128, DM], F32)
        self.Bgb = Buf()
        ph.dma("sp", self.gam[:], g_row.partition_broadcast(128), writes=[self.Bgb])
        ph.dma("sp", self.bet[:], b_row.partition_broadcast(128), writes=[self.Bgb])
        self.eps = ph.sb(tag + "_eps", [128, 1], F32)
        self.Beps = Buf()
        ph.memset("dve", self.eps[:], LN_EPS, [self.Beps])
        self.junk = [ph.sb(tag + "_junk%d" % i, [128, DM], F32) for i in range(nslot)]
        self.Bj = [Buf() for _ in range(nslot)]
        self.st = [ph.sb(tag + "_st%d" % i, [128, 8], F32) for i in range(nslot)]
        self.Bst = [Buf() for _ in range(nslot)]
        self.k = 0

    def run(self, z, Bz, out, Bout):
        ph = self.ph
        i = self.k % self.ns
        self.k += 1
        st, Bst, junk, Bj = self.st[i], self.Bst[i], self.junk[i], self.Bj[i]
        ph.act(junk[:], z, AF.Identity, [Bz], [Bj, Bst], accum=st[:, 0:1])
        ph.act(junk[:], z, AF.Square, [Bz], [Bj, Bst], accum=st[:, 1:2])
        ph.ts("dve", st[:, 2:3], st[:, 0:1], 1.0 / DM, None, ALU.mult, None, [Bst], [Bst])
        ph.tt("dve", st[:, 3:4], st[:, 2:3], st[:, 2:3], ALU.mult, [Bst], [Bst])
        ph.stt("dve", st[:, 4:5], st[:, 1:2], 1.0 / DM, st[:, 3:4], ALU.mult, ALU.subtract, [Bst], [Bst])
        ph.act(st[:, 5:6], st[:, 4:5], AF.Sqrt, [Bst, self.Beps], [Bst], bias=self.eps[:, 0:1])
        ph.stt("dve", junk[:], z, st[:, 2:3], self.gam[:], ALU.subtract, ALU.mult, [Bz, Bst, self.Bgb], [Bj])
        ph.recip(st[:, 6:7], st[:, 5:6], [Bst], [Bst])
        ph.stt("dve", out, junk[:], st[:, 6:7], self.bet[:], ALU.mult, ALU.add, [Bj, Bst, self.Bgb], [Bout])


class TrCtx:
    def __init__(self, ph, ident, Bid, tag="tr"):
        self.ph = ph
        self.ident = ident
        self.Bid = Bid
        self.pst = ph.ps(tag + "_ps", [128, DM], BF16)
        self.Bps = Buf()

    def run(self, src, Bsrc, dst, Bdst, eng="act"):
        ph = self.ph
        for j in range(8):
            ph.tr(self.pst[:, j * 128:(j + 1) * 128], src[:, j * 128:(j + 1) * 128], self.ident[:], [Bsrc, self.Bid], [self.Bps])
        ph.cp(eng, dst, self.pst[:], [self.Bps], [Bdst])


def load_ident_bf(ph, T):
    idb = ph.sb("identb", [128, 128], BF16)
    Bid = Buf()
    ph.dma("pool", idb[:], T["ident"], writes=[Bid])
    return idb, Bid


FM_SRC = (
    [(c * 128, None) for c in range(4)]
    + [(512, 512), (576, 576)]
    + [(768 + c * 128, None) for c in range(4)]
    + [(1280, 1280), (1344, 1344)]
    + [(1408, 1408), (1472, 1472)]
    + [(1536, 1536), (1600, 1600)]
    + [(1792, 1792), (1856, 1856)]
)
Q_CH = (0, 1, 2, 3, 6, 7, 8, 9)
SH_CH = (10, 11, 12, 13)
TM_SRC = ((640, 128), (1664, 128), (1920, 128), (2048, 24))


def phase_inproj(g, T, S, b):
    ph = Phase(g, "ipj%d" % b)
    wfm = ph.sb("wfm", [128, 8, 18 * 128], BF16)
    wtm = ph.sb("wtm", [128, 8, 408], BF16)
    Bwc = [[Buf(), Buf()] for _ in range(18)]
    Bwt = [Buf() for _ in range(4)]
    w_in = T["att_w_in"].rearrange("(ko p) n -> p ko n", p=128)
    xs = [ph.sb("xs%d" % i, [128, 8, 512], BF16) for i in range(2)]
    Bx = [Buf(), Buf()]
    xT_d0 = T["xT"].rearrange("(ko p) t -> p ko t", p=128)
    for c, (c0, dup) in enumerate(FM_SRC):
        if dup is None:
            ph.dma("pool", wfm[:, :, c * 128:(c + 1) * 128], w_in[:, :, c0:c0 + 128], writes=[Bwc[c][0]])
        else:
            ph.dma("pool", wfm[:, :, c * 128:c * 128 + 64], w_in[:, :, c0:c0 + 64], writes=[Bwc[c][0]])
            ph.dma("pool", wfm[:, :, c * 128 + 64:c * 128 + 128], w_in[:, :, c0:c0 + 64], writes=[Bwc[c][1]])
    o = 0
    for i4, (c0, w) in enumerate(TM_SRC):
        ph.dma("pool", wtm[:, :, o:o + w], w_in[:, :, c0:c0 + w], writes=[Bwt[i4]])
        o += w
    psf = [ph.ps("psf%d" % i, [128, 512], F32) for i in range(4)]
    Bpf = [Buf() for _ in range(4)]
    pst = [ph.ps("pst%d" % i, [128, 512], F32) for i in range(2)]
    Bpt = [Buf() for _ in range(2)]
    xT_d = T["xT"].rearrange("(ko p) t -> p ko t", p=128)
    k = 0
    for s in range(4):
        t0 = b * SEQ + s * 512
        ls = s * 512
        x_, Bx_ = xs[s % 2], Bx[s % 2]
        ph.dma("pool", x_[:], xT_d[:, :, t0:t0 + 512], writes=[Bx_])
        for c in range(18):
            p_, Bp_ = psf[k % 4], Bpf[k % 4]
            k += 1
            for ko in range(8):
                ph.mm(p_[:], wfm[:, ko, c * 128:(c + 1) * 128], x_[:, ko, :], ko == 0, ko == 7, Bwc[c] + [Bx_], [Bp_])
            Bd = S["Bfm"][c][s]
            if c in Q_CH:
                if c % 2 == 0:
                    ph.act(S["fm"][:, c, ls:ls + 512], p_[:], AF.Copy, [Bp_], [Bd], scale=0.125)
                else:
                    ph.ts("dve", S["fm"][:, c, ls:ls + 512], p_[:], 0.125, None, ALU.mult, None, [Bp_], [Bd])
            elif c in SH_CH:
                ph.cp("act", S["fm"][0:64, c, ls:ls + 512], p_[0:64, :], [Bp_], [Bd])
                wr = [Bd] if s == 0 else [Bd, S["Bfm"][c][s - 1]]
                if s == 0:
                    ph.cp("dve", S["fm"][64:128, c, 0:511], p_[64:128, 1:512], [Bp_], wr)
                else:
                    ph.cp("dve", S["fm"][64:128, c, ls - 1:ls + 511], p_[64:128, :], [Bp_], wr)
            else:
                if c % 2 == 0:
                    ph.cp("act", S["fm"][:, c, ls:ls + 512], p_[:], [Bp_], [Bd])
                else:
                    ph.cp("dve", S["fm"][:, c, ls:ls + 512], p_[:], [Bp_], [Bd])
        for tt in range(4):
            tile = s * 4 + tt
            p_, Bp_ = pst[tile % 2], Bpt[tile % 2]
            for ko in range(8):
                ph.mm(p_[:, 0:408], x_[:, ko, tt * 128:(tt + 1) * 128], wtm[:, ko, :], ko == 0, ko == 7, Bwt + [Bx_], [Bp_])
            ph.cp("dve", S["vtm"][:, tile, :], p_[:, 0:384], [Bp_], [S["Bv"][tile]])
            ph.act(S["gt"][:, tile, :], p_[:, 384:408], AF.Sigmoid, [Bp_], [S["Bg"][tile]])
    ph.finish()


def phase_cmp(g, T, S, b):
    ph = Phase(g, "cmp%d" % b)
    fm, Bfm = S["fm"], S["Bfm"]
    kcT, vc, Bkc, Bvc = S["kcT"], S["vc"], S["Bkc"], S["Bvc"]
    Bcw = Buf()
    w1, w2, pe = {}, {}, {}
    for kind in ("k", "v"):
        w1[kind] = ph.sb("w1" + kind, [128, 16, 256], BF16)
        ph.dma("pool", w1[kind][:], T["cmp_w%s1" % kind].rearrange("(lp p) h -> p lp h", p=128), writes=[Bcw])
        pe[kind] = ph.sb("pe" + kind, [128, 16], BF16)
        ph.dma("pool", pe[kind][:], T["cmp_pe_" + kind], writes=[Bcw])
    w2["k"] = ph.sb("w2k", [128, 2, 128], BF16)
    w2k_d = T["cmp_wk2"].rearrange("(hc p) d -> p hc d", p=128)
    ph.dma("pool", w2["k"][:, :, 0:64], w2k_d, writes=[Bcw])
    ph.dma("pool", w2["k"][:, :, 64:128], w2k_d, writes=[Bcw])
    w2["v"] = ph.sb("w2v", [128, 2, 64], BF16)
    ph.dma("pool", w2["v"][:], T["cmp_wv2"].rearrange("(hc p) d -> p hc d", p=128), writes=[Bcw])
    ph.memset("dve", kcT[:], 0.0, Bkc)
    ph.memset("dve", vc[:], 0.0, Bvc)
    hT = ph.sb("hT", [128, 2, 128], BF16)
    BhT = Buf()
    cbias = ph.sb("cbias", [128, 4], F32)
    Bcb = Buf()
    PS_S = ph.ps("S", [128, 512], F32)
    BSs = Buf()
    PS_O = ph.ps("O", [128, 64], F32)
    BO = Buf()
    allslab = lambda c: [Bfm[c][s] for s in range(4)]
    for ki, kind in enumerate(("k", "v")):
        for hc in range(2):
            for lp in range(16):
                ph.mm(PS_S[:, hc:hc + 1], w1[kind][:, lp, hc * 128:(hc + 1) * 128], pe[kind][:, lp:lp + 1], lp == 0, lp == 15, [Bcw], [BSs])
        ph.cp("dve", cbias[:, ki * 2:ki * 2 + 2], PS_S[:, 0:2], [BSs], [Bcb])
        for kv in range(2):
            c = (10 if kind == "k" else 12) + kv
            for hc in range(2):
                for lp in range(16):
                    ph.mm(PS_S[:, 0:127], w1[kind][:, lp, hc * 128:(hc + 1) * 128], fm[:, c, 2 * lp:2 * lp + 16 * 126 + 1:16],
                          lp == 0, lp == 15, [Bcw] + allslab(c), [BSs])
                ph.act(hT[:, hc, 0:127], PS_S[:, 0:127], AF.Gelu_apprx_tanh, [BSs, Bcb], [BhT], bias=cbias[:, ki * 2 + hc:ki * 2 + hc + 1])
            if kind == "k":
                po = PS_S[:, 128:255]
                for hc in range(2):
                    ph.mm(po, w2["k"][:, hc, :], hT[:, hc, 0:127], hc == 0, hc == 1, [Bcw, BhT], [BSs])
                ph.cp("dve", kcT[:, kv, 0:127], po, [BSs], [Bkc[kv]])
            else:
                for hc in range(2):
                    ph.mm(PS_O[0:127, :], hT[:, hc, 0:127], w2["v"][:, hc, :], hc == 0, hc == 1, [Bcw, BhT], [BO])
                ph.cp("dve", vc[0:127, kv, :], PS_O[0:127, :], [BO], [Bvc[kv]])
    ph.finish()


def phase_attn(g, T, S, b):
    ph = Phase(g, "att%d" % b)
    fm, Bfm, vtm, Bv, gt, Bg = S["fm"], S["Bfm"], S["vtm"], S["Bv"], S["gt"], S["Bg"]
    kcT, vc, Bkc, Bvc = S["kcT"], S["vc"], S["Bkc"], S["Bvc"]
    Bc = Buf()
    BA = ph.sb("BA", [128, 8, 256], F32)
    BW = ph.sb("BW", [128, 8, 384], F32)
    BC = ph.sb("BC", [128, 8, 248], F32)
    D_S = ph.sb("D_S", [128, 2048], F32)
    FMt = ph.sb("FM", [128, 62], F32)
    sink = ph.sb("sink", [128, 8], F32)
    for t_, n_ in ((BA, "BA"), (BW, "BW"), (BC, "BC"), (D_S, "D_S"), (FMt, "FM")):
        ph.dma("sp", t_[:], T[n_], writes=[Bc])
    ph.dma("sp", sink[:], T["att_sinks"].partition_broadcast(128), writes=[Bc])
    idb, Bid = load_ident_bf(ph, T)
    PS_big = ph.ps("big", [128, 2048], F32)
    Bbig = Buf()
    PS_T = ph.ps("T", [128, 2048], BF16)
    BT = Buf()
    PS_O = [ph.ps("O%d" % i, [128, 512], F32) for i in range(2)]
    BO = [Buf(), Buf()]
    NW = 4
    sbt = [ph.sb("sbt%d" % i, [128, 2048], F32) for i in range(NW)]
    E = [ph.sb("E%d" % i, [128, 2048], BF16) for i in range(NW)]
    PTs = [ph.sb("PTs%d" % i, [128, 2048], BF16) for i in range(NW)]
    Bsb = [Buf() for _ in range(NW)]
    BE = [Buf() for _ in range(NW)]
    BPT = [Buf() for _ in range(NW)]
    NST = 8
    stl = [ph.sb("st%d" % i, [128, 64], F32) for i in range(NST)]
    Bstl = [Buf() for _ in range(NST)]
    cnt = {"w": 0, "st": 0, "o": 0}
    pn = ph.sb("pn", [128, 1024], F32)
    Bpn = Buf()
    pacc = ph.sb("pacc", [128, 2, 132], F32)
    Bpacc = Buf()
    imp = ph.sb("imp", [128, 2, 32], F32)
    top8 = ph.sb("top8", [128, 2, 8], F32)
    madd = ph.sb("madd", [128, 2, 32], F32)
    Bsel = Buf()
    oacc = ph.sb("oacc", [128, 512], F32)
    Boacc = Buf()
    otmp = ph.sb("otmp", [128, 256], F32)
    Botmp = Buf()
    ocomb = [ph.sb("ocomb%d" % i, [128, DM], BF16) for i in range(2)]
    Bocomb = [Buf(), Buf()]
    OC = T["OC"]
    ph.memset("dve", pacc[:], 0.0, [Bpacc])

    def slabs(c, a, bnd):
        return [Bfm[c][s] for s in range(a // 512, (bnd - 1) // 512 + 1)]

    def sep(dst):
        ph.mm(dst, idb[:, 0:128], idb[:, 0:1], True, True, [Bid], [Bbig])

    def chain_p1(G, L, score_fn, bias3, slope, extra_add, mx_sink, clamp, vfn, LS=None):
        i = cnt["w"] % NW
        cnt["w"] += 1
        j = cnt["st"] % NST
        cnt["st"] += 1
        oi = cnt["o"] % 2
        cnt["o"] += 1
        sb_, Bs_, E_, BE_, PT_, BPT_ = sbt[i], Bsb[i], E[i], BE[i], PTs[i], BPT[i]
        st, Bst = stl[j], Bstl[j]
        LS = L if LS is None else LS
        W = G * L
        nkb = L // 128
        score_fn()
        v3 = lambda ap: ap[:, 0:G * LS].rearrange("p (g l) -> p g l", l=LS)[:, :, 0:L]
        if bias3 is not None:
            ph.tt("dve", v3(sb_), v3(PS_big), bias3, ALU.add, [Bbig, Bc], [Bs_])
        else:
            ph.stt("dve", sb_[:, 0:W], D_S[:, 2048 - W:2048], -slope, PS_big[:, 0:W], ALU.mult, ALU.add, [Bc, Bbig], [Bs_])
        if extra_add is not None:
            nb = W // 64
            ph.tt("dve", sb_[:, 0:W].rearrange("p (j k) -> p j k", k=64), sb_[:, 0:W].rearrange("p (j k) -> p j k", k=64),
                  extra_add.unsqueeze(2).to_broadcast([128, nb, 64]), ALU.add, [Bs_, Bsel], [Bs_])
        ph.op("dve", lambda e, o=st[:, 0:G], a=v3(sb_): e.reduce_max(out=o, in_=a, axis=AX.X), [Bs_], [Bst])
        if mx_sink:
            ph.tt("dve", st[:, 0:G], st[:, 0:G], sink[:, 0:G], ALU.max, [Bst, Bc], [Bst])
        if clamp:
            ph.ts("dve", st[:, 0:G], st[:, 0:G], -50.0, None, ALU.max, None, [Bst], [Bst])
        ph.ts("dve", st[:, 48:48 + G], st[:, 0:G], -1.0, None, ALU.mult, None, [Bst], [Bst])
        for gi in range(G):
            ph.act(E_[:, gi * LS:gi * LS + L], sb_[:, gi * LS:gi * LS + L], AF.Exp, [Bs_, Bst], [BE_, Bst],
                   bias=st[:, 48 + gi:49 + gi], accum=st[:, 8 + gi:9 + gi])
        if mx_sink:
            ph.tt("dve", st[:, 24:24 + G], sink[:, 0:G], st[:, 0:G], ALU.subtract, [Bst, Bc], [Bst])
            ph.act(st[:, 32:32 + G], st[:, 24:24 + G], AF.Exp, [Bst], [Bst])
            ph.tt("dve", st[:, 8:8 + G], st[:, 8:8 + G], st[:, 32:32 + G], ALU.add, [Bst], [Bst])
        if clamp:
            ph.ts("dve", st[:, 8:8 + G], st[:, 8:8 + G], 1e-30, None, ALU.max, None, [Bst], [Bst])
        ph.recip(st[:, 16:16 + G], st[:, 8:8 + G], [Bst], [Bst])
        return dict(G=G, L=L, LS=LS, W=W, nkb=nkb, oi=oi, E=E_, BE=BE_, PT=PT_, BPT=BPT_, st=st, Bst=Bst, vfn=vfn,
                    po=PS_O[oi], Bpo=BO[oi])

    def chain_p2(c):
        G, L, LS, W, nkb, oi = c["G"], c["L"], c["LS"], c["W"], c["nkb"], c["oi"]
        E_, BE_, PT_, BPT_ = c["E"], c["BE"], c["PT"], c["BPT"]
        for k in range(G * nkb):
            eo = (k // nkb) * LS + (k % nkb) * 128
            ph.tr(PS_T[:, k * 128:(k + 1) * 128], E_[:, eo:eo + 128], idb[:], [BE_, Bid], [BT])
        ph.cp("act", PT_[:, 0:W], PS_T[:, 0:W], [BT], [BPT_])
        for gi in range(G):
            for jb in range(nkb):
                rhs, Br = c["vfn"](gi, jb)
                ph.mm(PS_O[oi][:, gi * 64:(gi + 1) * 64], PT_[:, (gi * nkb + jb) * 128:(gi * nkb + jb + 1) * 128], rhs,
                      jb == 0, jb == nkb - 1, [BPT_] + Br, [BO[oi]])

    pend = []

    def submit(c, post_fn):
        if len(pend) >= 2:
            pend.pop(0)()

        def later(c=c, post_fn=post_fn):
            chain_p2(c)
            post_fn(c)
        pend.append(later)

    for n in range(16):
        q0 = n * 128
        oc_, Boc_ = ocomb[n % 2], Bocomb[n % 2]
        j0 = max(0, n - 1)
        L = (n - j0 + 1) * 128
        k0 = j0 * 128

        def scoreA(L=L, k0=k0, n=n, q0=q0):
            for h in (0, 2, 4, 6, 1, 3, 5, 7):
                kv, qc, half = h // 4, h // 2, h % 2
                hp = slice(half * 64, half * 64 + 64)
                if h == 1:
                    sep(PS_big[:, h * L:h * L + 1])
                ph.mm(PS_big[:, h * L:(h + 1) * L], fm[hp, qc, q0:q0 + 128], fm[hp, 4 + kv, k0:k0 + L], True, True,
                      [Bfm[qc][n // 4]] + slabs(4 + kv, k0, k0 + L), [Bbig])
        c = chain_p1(8, L, scoreA, BA[:, :, 256 - L:256], None, None, True, False,
                     lambda gi, jb, j0=j0: (vtm[:, j0 + jb, (gi // 4) * 64:(gi // 4 + 1) * 64], [Bv[j0 + jb]]))

        def postA(c, oc_=oc_, Boc_=Boc_):
            ph.tt("dve", oc_[:, 0:512].rearrange("p (g d) -> p g d", d=64), c["po"][:, 0:512].rearrange("p (g d) -> p g d", d=64),
                  c["st"][:, 16:24].unsqueeze(2).to_broadcast([128, 8, 64]), ALU.mult, [c["Bpo"], c["Bst"]], [Boc_])
        submit(c, postA)

        def scoreC(n=n, q0=q0):
            for hb in (0, 2, 4, 6, 1, 3, 5, 7):
                kv, qc, half = hb // 4, 6 + hb // 2, hb % 2
                hp = slice(half * 64, half * 64 + 64)
                if hb == 1:
                    sep(PS_big[:, hb * 128:hb * 128 + 1])
                ph.mm(PS_big[:, hb * 128:(hb + 1) * 128], fm[hp, qc, q0:q0 + 128], kcT[hp, kv, :], True, True, [Bfm[qc][n // 4], Bkc[kv]], [Bbig])
        c = chain_p1(8, 128, scoreC, BC[:, :, 120 - 8 * n:248 - 8 * n], None, None, False, n == 0,
                     lambda gi, jb: (vc[:, gi // 4, :], [Bvc[gi // 4]]))
        st, Bst = c["st"], c["Bst"]
        ph.tt("dve", pn[:].rearrange("p (g c) -> p g c", c=128), c["E"][:, 0:1024].rearrange("p (g c) -> p g c", c=128),
              st[:, 16:24].unsqueeze(2).to_broadcast([128, 8, 128]), ALU.mult, [c["BE"], Bst], [Bpn])
        ph.op("dve", lambda e: e.reduce_sum(out=pacc[:, :, 1:129], in_=pn[:].rearrange("p (k g c) -> p k c g", k=2, g=4), axis=AX.X), [Bpn], [Bpacc])
        ph.tt("dve", imp[:], pacc[:, :, 0:125:4], pacc[:, :, 1:126:4], ALU.add, [Bpacc], [Bsel])
        for i2 in (2, 3, 4):
            ph.tt("dve", imp[:], imp[:], pacc[:, :, i2:i2 + 125:4], ALU.add, [Bpacc, Bsel], [Bsel])
        ph.tt("dve", imp[:], imp[:], FMt[:, 30 - 2 * n:62 - 2 * n].unsqueeze(1).to_broadcast([128, 2, 32]), ALU.add, [Bsel, Bc], [Bsel])
        ph.memset("dve", imp[:, :, 0:1], 3.0e9, [Bsel])
        for kv in range(2):
            ph.op("dve", lambda e, kv=kv: e.max(out=top8[:, kv, :], in_=imp[:, kv, :]), [Bsel], [Bsel])
        for kv in range(2):
            ph.ts("dve", madd[:, kv, :], imp[:, kv, :], top8[:, kv, 7:8], 3.0e4, ALU.is_ge, ALU.mult, [Bsel], [Bsel])
        ph.ts("dve", madd[:], madd[:], -3.0e4, None, ALU.add, None, [Bsel], [Bsel])

        def postC(c, n=n):
            st, Bst = c["st"], c["Bst"]
            ph.tt("dve", st[:, 40:48], st[:, 16:24], gt[:, n, 0:24:3], ALU.mult, [Bst, Bg[n]], [Bst])
            ph.tt("dve", oacc[:].rearrange("p (g d) -> p g d", d=64), c["po"][:, 0:512].rearrange("p (g d) -> p g d", d=64),
                  st[:, 40:48].unsqueeze(2).to_broadcast([128, 8, 64]), ALU.mult, [c["Bpo"], Bst], [Boacc])
        submit(c, postC)

        j0 = max(0, n - 2)
        L = (n - j0 + 1) * 128
        k0 = j0 * 128
        for kv in range(2):
            def scoreW(kv=kv, L=L, k0=k0, n=n, q0=q0):
                for gi in (0, 2, 1, 3):
                    hb = kv * 4 + gi
                    qc, half = 6 + hb // 2, hb % 2
                    hp = slice(half * 64, half * 64 + 64)
                    LSw = 512
                    if gi == 1:
                        sep(PS_big[:, gi * LSw:gi * LSw + 1])
                    ph.mm(PS_big[:, gi * LSw:gi * LSw + L], fm[hp, qc, q0:q0 + 128], fm[hp, 16 + kv, k0:k0 + L], True, True,
                          [Bfm[qc][n // 4]] + slabs(16 + kv, k0, k0 + L), [Bbig])
            c = chain_p1(4, L, scoreW, BW[:, kv * 4:(kv + 1) * 4, 384 - L:384], None, None, False, False,
                         lambda gi, jb, j0=j0, kv=kv: (vtm[:, j0 + jb, 256 + kv * 64:256 + (kv + 1) * 64], [Bv[j0 + jb]]), LS=512)

            def postW(c, kv=kv, n=n):
                st, Bst = c["st"], c["Bst"]
                ph.tt("dve", st[:, 40:44], st[:, 16:20], gt[:, n, kv * 12 + 2:kv * 12 + 12:3], ALU.mult, [Bst, Bg[n]], [Bst])
                ph.tt("dve", otmp[:].rearrange("p (g d) -> p g d", d=64), c["po"][:, 0:256].rearrange("p (g d) -> p g d", d=64),
                      st[:, 40:44].unsqueeze(2).to_broadcast([128, 4, 64]), ALU.mult, [c["Bpo"], Bst], [Botmp])
                ph.tt("dve", oacc[:, kv * 256:(kv + 1) * 256], oacc[:, kv * 256:(kv + 1) * 256], otmp[:], ALU.add, [Botmp, Boacc], [Boacc])
            submit(c, postW)

        L = (n + 1) * 128
        for hb in range(8):
            kv, qc, half = hb // 4, 6 + hb // 2, hb % 2
            hp = slice(half * 64, half * 64 + 64)

            def scoreS(kv=kv, qc=qc, hp=hp, L=L, n=n, q0=q0):
                for c0 in range(0, L, 512):
                    w = min(512, L - c0)
                    ph.mm(PS_big[:, c0:c0 + w], fm[hp, qc, q0:q0 + 128], fm[hp, 14 + kv, c0:c0 + w], True, True,
                          [Bfm[qc][n // 4]] + slabs(14 + kv, c0, c0 + w), [Bbig])
            c = chain_p1(1, L, scoreS, None, SLOPES[8 + hb], madd[:, kv, 0:2 * (n + 1)], False, False,
                         lambda gi, jb, kv=kv: (vtm[:, jb, 128 + kv * 64:128 + (kv + 1) * 64], [Bv[jb]]))

            def postS(c, hb=hb, n=n, q0=q0, oc_=oc_, Boc_=Boc_):
                st, Bst = c["st"], c["Bst"]
                ph.tt("dve", st[:, 40:41], st[:, 16:17], gt[:, n, hb * 3 + 1:hb * 3 + 2], ALU.mult, [Bst, Bg[n]], [Bst])
                ph.stt("dve", oacc[:, hb * 64:(hb + 1) * 64], c["po"][:, 0:64], st[:, 40:41], oacc[:, hb * 64:(hb + 1) * 64], ALU.mult, ALU.add,
                       [c["Bpo"], Bst, Boacc], [Boacc])
                if hb == 7:
                    ph.cp("act", oc_[:, 512:1024], oacc[:], [Boacc], [Boc_])
                    tok0 = b * SEQ + q0
                    ph.dma("sp", OC[tok0:tok0 + 128, :], oc_[:], reads=[Boc_])
            submit(c, postS)
    while pend:
        pend.pop(0)()
    ph.finish()


class PostCtx:
    def __init__(self, ph, T, ln_idx, Xin, Xout, XTout, idb, Bid, tag="post", nslot=2, store_q="sp"):
        self.ph = ph
        self.ns = nslot
        self.sq = store_q
        self.ln = LNCtx(ph, T["ln_g"][ln_idx:ln_idx + 1, :], T["ln_b"][ln_idx:ln_idx + 1, :], tag + "ln", nslot)
        self.Xin, self.Xout, self.XTout = Xin, Xout, XTout
        self.xt = [ph.sb(tag + "_x%d" % i, [128, DM], F32) for i in range(nslot)]
        self.Bxt = [Buf() for _ in range(nslot)]
        self.z = [ph.sb(tag + "_z%d" % i, [128, DM], F32) for i in range(nslot)]
        self.Bz = [Buf() for _ in range(nslot)]
        self.xo = [ph.sb(tag + "_xo%d" % i, [128, DM], F32) for i in range(nslot)]
        self.Bxo = [Buf() for _ in range(nslot)]
        if XTout is not None:
            self.xb = [ph.sb(tag + "_xb%d" % i, [128, DM], BF16) for i in range(nslot)]
            self.Bxb = [Buf() for _ in range(nslot)]
            self.xT = [ph.sb(tag + "_xT%d" % i, [128, DM], BF16) for i in range(nslot)]
            self.BxT = [Buf() for _ in range(nslot)]
            self.trc = TrCtx(ph, idb, Bid, tag + "tr")
        self.k = 0

    def prefetch(self, tile):
        i = tile % self.ns
        self.ph.dma("sp", self.xt[i][:], self.Xin[tile * 128:(tile + 1) * 128, :], writes=[self.Bxt[i]])

    def run_a(self, tile, mix_parts, Bmix):
        ph = self.ph
        i = tile % self.ns
        xt, Bxt, z, Bz, xo, Bxo = self.xt[i], self.Bxt[i], self.z[i], self.Bz[i], self.xo[i], self.Bxo[i]
        for c0, w, ap in mix_parts:
            ph.stt("dve", z[:, c0:c0 + w], xt[:, c0:c0 + w], ALPHA, ap, ALU.mult, ALU.add, [Bxt] + Bmix, [Bz])
        self.ln.run(z[:], Bz, xo[:], Bxo)
        ph.dma(self.sq, self.Xout[tile * 128:(tile + 1) * 128, :], xo[:], reads=[Bxo])
        return xo, Bxo

    def run_b(self, tile):
        if self.XTout is None:
            return
        ph = self.ph
        i = tile % self.ns
        xo, Bxo = self.xo[i], self.Bxo[i]
        xb, Bxb, xT, BxT = self.xb[i], self.Bxb[i], self.xT[i], self.BxT[i]
        ph.cp("pool", xb[:], xo[:], [Bxo], [Bxb])
        self.trc.run(xb, Bxb, xT[:], BxT, "act")
        ph.dma(self.sq, self.XTout.rearrange("(ko p) t -> p ko t", p=128)[:, :, tile * 128:(tile + 1) * 128],
               xT[:].rearrange("p (ko t) -> p ko t", t=128), reads=[BxT])

    def run(self, tile, mix_parts, Bmix):
        r = self.run_a(tile, mix_parts, Bmix)
        self.run_b(tile)
        return r


def phase_oproj(g, T):
    ph = Phase(g, "opj")
    idb, Bid = load_ident_bf(ph, T)
    wo = ph.sb("wo", [128, 8, DM], BF16)
    Bwo = Buf()
    ph.dma("pool", wo[:], T["att_w_o"].rearrange("(ko p) n -> p ko n", p=128), writes=[Bwo])
    post = PostCtx(ph, T, 0, T["x"], T["X1"], T["X1T"], idb, Bid, nslot=4, store_q="pool")
    oc = [ph.sb("oc%d" % i, [128, DM], BF16) for i in range(4)]
    Boc = [Buf() for _ in range(4)]
    oT = [ph.sb("oT%d" % i, [128, DM], BF16) for i in range(4)]
    BoT = [Buf() for _ in range(4)]
    trc = TrCtx(ph, idb, Bid, "otr")
    psm = [ph.ps("psm%d" % i, [128, 512], F32) for i in range(4)]
    Bpm = [Buf() for _ in range(4)]
    for tile in range(32):
        i = tile % 4
        ph.dma("sp", oc[i][:], T["OC"][tile * 128:(tile + 1) * 128, :], writes=[Boc[i]])
        post.prefetch(tile)
        trc.run(oc[i], Boc[i], oT[i][:], BoT[i], "act")
        parts = []
        Bm = []
        for ch in range(2):
            p_, Bp_ = psm[(tile * 2 + ch) % 4], Bpm[(tile * 2 + ch) % 4]
            for j in range(8):
                ph.mm(p_[:], oT[i][:, j * 128:(j + 1) * 128], wo[:, j, ch * 512:(ch + 1) * 512], j == 0, j == 7, [BoT[i], Bwo], [Bp_])
            parts.append((ch * 512, 512, p_[:]))
            Bm.append(Bp_)
        post.run_a(tile, parts, Bm)
        if tile > 1:
            post.run_b(tile - 2)
    post.run_b(30)
    post.run_b(31)
    ph.finish()


def phase_glu(g, T, name, XTin, Xin, experts, F, gates, ln_idx, Xout, XTout):
    G = 1024
    NT = G // 128
    nchunks = F // 128
    ftiles = [(c, min(4, nchunks - c)) for c in range(0, nchunks, 4)]
    batches = [ftiles[i:i + 3] for i in range(0, len(ftiles), 3)]
    ph = Phase(g, name)
    idb, Bid = (None, None)
    if XTout is not None:
        idb, Bid = load_ident_bf(ph, T)
    post = PostCtx(ph, T, ln_idx, Xin, Xout, XTout, idb, Bid)
    xTt = ph.sb("xTt", [128, 8, G], BF16)
    BxT = Buf()
    yacc = ph.sb("yacc", [128, NT, DM], F32)
    Bya = [Buf() for _ in range(NT)]
    hT = ph.sb("hT", [128, 12, G], BF16)
    BhT = [Buf() for _ in range(12)]
    wg = [ph.sb("wg%d" % i, [128, 8, 512], BF16) for i in range(2)]
    wu = [ph.sb("wu%d" % i, [128, 8, 512], BF16) for i in range(2)]
    Bwgu = [Buf(), Buf()]
    wd = [ph.sb("wd%d" % i, [128, 12, 512], BF16) for i in range(2)]
    Bwd = [Buf(), Buf()]
    sg = [ph.sb("sg%d" % i, [128, 512], F32) for i in range(2)]
    Bsg = [Buf(), Buf()]
    psg = [ph.ps("psg%d" % i, [128, 512], F32) for i in range(2)]
    psu = [ph.ps("psu%d" % i, [128, 512], F32) for i in range(2)]
    Bpgu = [Buf(), Buf()]
    psy = [ph.ps("psy%d" % i, [128, 512], F32) for i in range(2)]
    Bpy = [Buf(), Buf()]
    if gates is not None:
        gtile = ph.sb("gates", [128, NT, NEXP], F32)
        Bgt = Buf()
    XT_d = XTin.rearrange("(ko p) t -> p ko t", p=128)
    kw = 0
    kd = 0
    kp = 0
    ky = 0
    pending = []
    for grp in range(NTOK // G):
        tok0 = grp * G
        ph.dma("sp", xTt[:], XT_d[:, :, tok0:tok0 + G], writes=[BxT])
        if gates is not None:
            ph.dma("sp", gtile[:], gates[tok0:tok0 + G, :].rearrange("(nt p) e -> p nt e", p=128), writes=[Bgt])
        first = True
        for e, (wg_d, wu_d, wd_d) in enumerate(experts):
            wgv = wg_d.rearrange("(ko p) f -> p ko f", p=128)
            wuv = wu_d.rearrange("(ko p) f -> p ko f", p=128)
            wdv = wd_d.rearrange("(fc p) c -> p fc c", p=128)
            for batch in batches:
                fc0 = batch[0][0]
                nfc = sum(w for _, w in batch)
                for ft0, w in batch:
                    i = kw % 2
                    kw += 1
                    ph.dma("pool", wg[i][:, :, 0:w * 128], wgv[:, :, ft0 * 128:(ft0 + w) * 128], writes=[Bwgu[i]])
                    ph.dma("pool", wu[i][:, :, 0:w * 128], wuv[:, :, ft0 * 128:(ft0 + w) * 128], writes=[Bwgu[i]])
                    for fcl in range(w):
                        fl = ft0 + fcl - fc0
                        for th in range(G // 512):
                            j = kp % 2
                            kp += 1
                            for ko in range(8):
                                ph.mm(psg[j][:], wg[i][:, ko, fcl * 128:(fcl + 1) * 128], xTt[:, ko, th * 512:(th + 1) * 512],
                                      ko == 0, ko == 7, [Bwgu[i], BxT], [Bpgu[j]])
                            for ko in range(8):
                                ph.mm(psu[j][:], wu[i][:, ko, fcl * 128:(fcl + 1) * 128], xTt[:, ko, th * 512:(th + 1) * 512],
                                      ko == 0, ko == 7, [Bwgu[i], BxT], [Bpgu[j]])
                            ph.act(sg[j][:], psg[j][:], AF.Silu, [Bpgu[j]], [Bsg[j]])
                            ph.tt("dve", hT[:, fl, th * 512:(th + 1) * 512], sg[j][:], psu[j][:], ALU.mult, [Bsg[j], Bpgu[j]], [BhT[fl]])
                        if pending:
                            pending.pop(0)()
                while pending:
                    pending.pop(0)()
                for ch in range(2):
                    i = kd % 2
                    kd += 1
                    ph.dma("pool", wd[i][:, 0:nfc, :], wdv[:, fc0:fc0 + nfc, ch * 512:(ch + 1) * 512], writes=[Bwd[i]])
                    for tt in range(NT):
                        j = ky % 2
                        ky += 1
                        for fl in range(nfc):
                            ph.mm(psy[j][:], hT[:, fl, tt * 128:(tt + 1) * 128], wd[i][:, fl, :], fl == 0, fl == nfc - 1,
                                  [BhT[fl], Bwd[i]], [Bpy[j]])
                        ya = yacc[:, tt, ch * 512:(ch + 1) * 512]
                        if gates is None:
                            if first:
                                ph.cp("dve", ya, psy[j][:], [Bpy[j]], [Bya[tt]])
                            else:
                                ph.tt("dve", ya, ya, psy[j][:], ALU.add, [Bpy[j], Bya[tt]], [Bya[tt]])
                        else:
                            gs = gtile[:, tt, e:e + 1]
                            if first:
                                ph.ts("dve", ya, psy[j][:], gs, None, ALU.mult, None, [Bpy[j], Bgt], [Bya[tt]])
                            else:
                                ph.stt("dve", ya, psy[j][:], gs, ya, ALU.mult, ALU.add, [Bpy[j], Bgt, Bya[tt]], [Bya[tt]])
                first = False
        for tt in range(NT):
            tile = grp * NT + tt

            def fin_a(tile=tile, tt=tt):
                post.prefetch(tile)
                post.run_a(tile, [(0, DM, yacc[:, tt, :])], [Bya[tt]])

            def fin_b(tile=tile):
                post.run_b(tile)
            pending.append(fin_a)
            if tt >= 1:
                pending.append(prev_b)
            prev_b = fin_b
        pending.append(prev_b)
    while pending:
        pending.pop(0)()
    ph.finish()


def phase_gmlp(g, T):
    import os
    GCUT = int(os.environ.get("GCUT", "9"))
    ph = Phase(g, "gmlp")
    idb, Bid = load_ident_bf(ph, T)
    Bw = Buf()
    win = ph.sb("win", [128, 8, 2048], BF16)
    ph.dma("pool", win[:], T["gmlp_w_in"].rearrange("(ko p) n -> p ko n", p=128), writes=[Bw])
    wout = ph.sb("wout", [128, 8, DM], BF16)
    ph.dma("pool", wout[:], T["gmlp_w_out"].rearrange("(ko p) n -> p ko n", p=128), writes=[Bw])
    wsf = ph.sb("wsf", [128, 8, 128], F32)
    ph.dma("sp", wsf[:], T["gmlp_wsT"], writes=[Bw])
    tril = ph.sb("tril", [128, 128], F32)
    ph.dma("sp", tril[:], T["TRIL"], writes=[Bw])
    wsT = ph.sb("wsTb", [128, 8, 128], BF16)
    Bws = Buf()
    ph.tt("dve", wsT[:], wsf[:], tril[:].unsqueeze(1).to_broadcast([128, 8, 128]), ALU.mult, [Bw], [Bws])
    bsT = ph.sb("bsTt", [128, 8], F32)
    ph.dma("sp", bsT[:], T["gmlp_bsT"], writes=[Bw])
    wr = ph.sb("wr", [128, 8, 8], F32)
    ph.dma("sp", wr[:], T["moe_w_router"].rearrange("(ko p) e -> p ko e", p=128), writes=[Bw])
    vln = LNCtx(ph, T["gmlp_ln_g"], T["gmlp_ln_b"], "vln", 3)
    post = PostCtx(ph, T, 2, T["X2"], T["X3"], None, idb, Bid, nslot=3, store_q="pool")
    x2T = [ph.sb("x2T%d" % i, [128, 8, 128], BF16) for i in range(2)]
    Bx2T = [Buf(), Buf()]
    u = [ph.sb("u%d" % i, [128, DM], BF16) for i in range(2)]
    v = [ph.sb("v%d" % i, [128, DM], F32) for i in range(2)]
    Bu, Bv_ = [Buf(), Buf()], [Buf(), Buf()]
    vn = [ph.sb("vn%d" % i, [128, DM], F32) for i in range(2)]
    Bvn = [Buf(), Buf()]
    vnb = [ph.sb("vnb%d" % i, [128, DM], BF16) for i in range(2)]
    Bvnb = [Buf(), Buf()]
    gated = ph.sb("gated", [128, DM], BF16)
    Bgd = Buf()
    gT = ph.sb("gT", [128, DM], BF16)
    BgT = Buf()
    x3Tl = ph.sb("x3Tl", [128, DM], BF16)
    Bx3Tl = Buf()
    xhi = ph.sb("xhi", [128, DM], BF16)
    Bxhi = Buf()
    xlo = ph.sb("xlo", [128, DM], BF16)
    Bxlo = Buf()
    wrh = ph.sb("wrh", [128, 8, 8], BF16)
    wrl = ph.sb("wrl", [128, 8, 8], BF16)
    Bwr = Buf()
    ph.cp("dve", wrh[:], wr[:], [Bw], [Bwr])
    ph.tt("dve", wrl[:], wr[:], wrh[:], ALU.subtract, [Bw, Bwr], [Bwr])
    x3Tb = ph.sb("x3Tb", [128, DM], BF16)
    Bx3Tb = Buf()
    rst = ph.sb("rst", [128, 32], F32)
    Brst = Buf()
    gout = ph.sb("gout", [128, 8], F32)
    Bgout = Buf()
    PSA = ph.ps("A", [128, 2048], F32)
    BA = [Buf() for _ in range(4)]
    PSB = ph.ps("B", [128, DM], F32)
    BB = [Buf(), Buf()]
    PSR = ph.ps("R", [128, 8], F32)
    BR = Buf()
    trc = TrCtx(ph, idb, Bid, "gtr")
    X2T_d = T["X2T"].rearrange("(ko p) t -> p ko t", p=128)
    X3T_d = T["X3T"].rearrange("(ko p) t -> p ko t", p=128)
    xo_of = {}

    def s1(tile):
        i = tile % 2
        ph.dma("sp", x2T[i][:], X2T_d[:, :, tile * 128:(tile + 1) * 128], writes=[Bx2T[i]])
        for cg in range(4):
            for ko in range(8):
                ph.mm(PSA[:, cg * 512:(cg + 1) * 512], x2T[i][:, ko, :], win[:, ko, cg * 512:(cg + 1) * 512], ko == 0, ko == 7, [Bx2T[i], Bw], [BA[cg]])
        ph.act(u[i][:, 0:512], PSA[:, 0:512], AF.Gelu_apprx_tanh, [BA[0]], [Bu[i]])
        ph.act(u[i][:, 512:1024], PSA[:, 512:1024], AF.Gelu_apprx_tanh, [BA[1]], [Bu[i]])
        ph.act(v[i][:, 0:512], PSA[:, 1024:1536], AF.Gelu_apprx_tanh, [BA[2]], [Bv_[i]])
        ph.act(v[i][:, 512:1024], PSA[:, 1536:2048], AF.Gelu_apprx_tanh, [BA[3]], [Bv_[i]])
        vln.run(v[i][:], Bv_[i], vn[i][:], Bvn[i])
        ph.cp("pool", vnb[i][:], vn[i][:], [Bvn[i]], [Bvnb[i]])

    def s2a(tile):
        i = tile % 2
        post.prefetch(tile)
        for gi in range(8):
            ph.mm(PSB[:, gi * 128:(gi + 1) * 128], wsT[:, gi, :], vnb[i][:, gi * 128:(gi + 1) * 128], True, True, [Bws, Bvnb[i]], [BB[gi // 4]])
        for gi in range(8):
            ph.stt("dve", gated[:, gi * 128:(gi + 1) * 128], PSB[:, gi * 128:(gi + 1) * 128], bsT[:, gi:gi + 1], u[i][:, gi * 128:(gi + 1) * 128],
                   ALU.add, ALU.mult, [BB[gi // 4], Bw, Bu[i]], [Bgd])
        trc.run(gated, Bgd, gT[:], BgT, "act")
        for ch in range(2):
            for j in range(8):
                ph.mm(PSB[:, ch * 512:(ch + 1) * 512], gT[:, j * 128:(j + 1) * 128], wout[:, j, ch * 512:(ch + 1) * 512], j == 0, j == 7, [BgT, Bw], [BB[ch]])
        xo_of[tile] = post.run_a(tile, [(0, 512, PSB[:, 0:512]), (512, 512, PSB[:, 512:1024])], [BB[0], BB[1]])

    def s2b(tile):
        xo, Bxo = xo_of.pop(tile)
        ph.cp("pool", xhi[:], xo[:], [Bxo], [Bxhi])
        ph.tt("dve", xlo[:], xo[:], xhi[:], ALU.subtract, [Bxo, Bxhi], [Bxlo])
        trc.run(xhi, Bxhi, x3Tb[:], Bx3Tb, "act")
        trc.run(xlo, Bxlo, x3Tl[:], Bx3Tl, "act")
        ph.dma("pool", X3T_d[:, :, tile * 128:(tile + 1) * 128], x3Tb[:].rearrange("p (ko t) -> p ko t", t=128), reads=[Bx3Tb])
        for j in range(8):
            ph.mm(PSR[:], x3Tb[:, j * 128:(j + 1) * 128], wrh[:, j, :], j == 0, False, [Bx3Tb, Bwr], [BR])
            ph.mm(PSR[:], x3Tb[:, j * 128:(j + 1) * 128], wrl[:, j, :], False, False, [Bx3Tb, Bwr], [BR])
            ph.mm(PSR[:], x3Tl[:, j * 128:(j + 1) * 128], wrh[:, j, :], False, j == 7, [Bx3Tl, Bwr], [BR])
        ph.cp("dve", rst[:, 0:8], PSR[:], [BR], [Brst])
        ph.op("dve", lambda e: e.max(out=rst[:, 8:16], in_=rst[:, 0:8]), [Brst], [Brst])
        ph.ts("dve", rst[:, 16:24], rst[:, 0:8], rst[:, 9:10], None, ALU.is_ge, None, [Brst], [Brst])
        ph.ts("dve", rst[:, 24:25], rst[:, 8:9], -1.0, None, ALU.mult, None, [Brst], [Brst])
        ph.act(rst[:, 0:8], rst[:, 0:8], AF.Exp, [Brst], [Brst], bias=rst[:, 24:25])
        ph.tt("dve", rst[:, 0:8], rst[:, 0:8], rst[:, 16:24], ALU.mult, [Brst], [Brst])
        ph.op("dve", lambda e: e.reduce_sum(out=rst[:, 25:26], in_=rst[:, 0:8], axis=AX.X), [Brst], [Brst])
        ph.recip(rst[:, 26:27], rst[:, 25:26], [Brst], [Brst])
        ph.ts("dve", gout[:], rst[:, 0:8], rst[:, 26:27], None, ALU.mult, None, [Brst], [Bgout])
        ph.dma("sp", T["GATES"][tile * 128:(tile + 1) * 128, :], gout[:], reads=[Bgout])

    NTL = 32
    s1(0)
    for tile in range(NTL):
        if tile + 1 < NTL:
            s1(tile + 1)
        s2a(tile)
        if tile >= 2:
            s2b(tile - 2)
    s2b(NTL - 2)
    s2b(NTL - 1)
    ph.finish()


IN_SPECS = [
    ("x", [NTOK, DM]), ("xT", [DM, NTOK]),
    ("att_w_in", [DM, 2072]), ("att_sinks", [1, 8]),
    ("cmp_pe_k", [128, 16]), ("cmp_wk1", [2048, 256]), ("cmp_wk2", [256, 64]),
    ("cmp_pe_v", [128, 16]), ("cmp_wv1", [2048, 256]), ("cmp_wv2", [256, 64]),
    ("att_w_o", [DM, DM]),
    ("ffn_w_gate", [DM, D_FF]), ("ffn_w_up", [DM, D_FF]), ("ffn_w_down", [D_FF, DM]),
    ("gmlp_w_in", [DM, 2048]), ("gmlp_ln_g", [1, DM]), ("gmlp_ln_b", [1, DM]),
    ("gmlp_wsT", [128, 8, 128]), ("gmlp_bsT", [128, 8]), ("gmlp_w_out", [DM, DM]),
    ("moe_w_router", [DM, 8]),
    ("moe_w_gate", [NEXP, DM, D_FFE]), ("moe_w_up", [NEXP, DM, D_FFE]), ("moe_w_down", [NEXP, D_FFE, DM]),
    ("ln_g", [4, DM]), ("ln_b", [4, DM]),
    ("ident", [128, 128]), ("D_A", [128, 256]), ("D_W", [128, 384]), ("D_S", [128, 2048]), ("D_C", [128, 248]),
    ("FM", [128, 62]), ("TRIL", [128, 128]),
    ("BA", [128, 8, 256]), ("BW", [128, 8, 384]), ("BC", [128, 8, 248]),
]

STAGES = ("attn", "oproj", "ffn", "gmlp", "moe")


def build_program(upto="moe", debug=False):
    nc = bass.Bass("TRN2", target_bir_lowering=False)
    T = {}
    for name, shape in IN_SPECS:
        T[name] = nc.dram_tensor(name, shape, F32, kind="ExternalInput").ap()
    T["y"] = nc.dram_tensor("y", [NTOK, DM], F32, kind="ExternalOutput").ap()
    dk = "ExternalOutput" if debug else "Internal"
    T["OC"] = nc.dram_tensor("OC", [NTOK, DM], BF16, kind=dk).ap()
    for nm in ("X1", "X2", "X3"):
        T[nm] = nc.dram_tensor(nm, [NTOK, DM], F32, kind=dk).ap()
        T[nm + "T"] = nc.dram_tensor(nm + "T", [DM, NTOK], BF16, kind=dk).ap()
    T["GATES"] = nc.dram_tensor("GATES", [NTOK, NEXP], F32, kind=dk).ap()
    gs = ExitStack()
    g = Glob(nc, gs)
    lvl = STAGES.index(upto)
    with ExitStack() as st:
        S = {}
        S["fm"] = st.enter_context(nc.sbuf_tensor("S_fm", [128, 18, SEQ], BF16))
        S["vtm"] = st.enter_context(nc.sbuf_tensor("S_vtm", [128, 16, 384], BF16))
        S["gt"] = st.enter_context(nc.sbuf_tensor("S_gt", [128, 16, 24], F32))
        S["kcT"] = st.enter_context(nc.sbuf_tensor("S_kcT", [128, 2, 128], BF16))
        S["vc"] = st.enter_context(nc.sbuf_tensor("S_vc", [128, 2, 64], BF16))
        for b in range(2):
            S["Bfm"] = [[Buf() for _ in range(4)] for _ in range(18)]
            S["Bv"] = [Buf() for _ in range(16)]
            S["Bg"] = [Buf() for _ in range(16)]
            S["Bkc"] = [Buf(), Buf()]
            S["Bvc"] = [Buf(), Buf()]
            phase_inproj(g, T, S, b)
            phase_cmp(g, T, S, b)
            phase_attn(g, T, S, b)
    if lvl >= 1:
        phase_oproj(g, T)
    if lvl >= 2:
        phase_glu(g, T, "ffn", T["X1T"], T["X1"], [(T["ffn_w_gate"], T["ffn_w_up"], T["ffn_w_down"])], D_FF, None, 1, T["X2"], T["X2T"])
    if lvl >= 3:
        phase_gmlp(g, T)
    if lvl >= 4:
        experts = [(T["moe_w_gate"][e], T["moe_w_up"][e], T["moe_w_down"][e]) for e in range(NEXP)]
        phase_glu(g, T, "moe", T["X3T"], T["X3"], experts, D_FFE, T["GATES"], 3, T["y"], None)
    gs.close()
    return nc


def host_consts():
    t = np.arange(128, dtype=np.float64)[:, None]

    def dtab(width, off, window):
        u = np.arange(width, dtype=np.float64)[None, :]
        d = t + off - u
        bad = d < 0
        if window is not None:
            bad |= d >= window
        return np.where(bad, MASKV, d).astype(np.float32)

    C = {}
    C["D_A"] = dtab(256, 128, 128)
    C["D_W"] = dtab(384, 256, 256)
    C["D_S"] = dtab(2048, 1920, None)
    v = np.arange(248, dtype=np.float64)[None, :]
    d = t + 1889 - 16 * v
    C["D_C"] = np.where(d < 0, MASKV, d).astype(np.float32)
    w = np.arange(62)[None, :] - 30
    hi = (np.arange(128)[:, None] >= 64).astype(np.int64)
    fmv = np.zeros((128, 62), np.float32)
    fmv[(w == hi) | (w == hi - 1)] = 1.0e9
    fmv[w > hi] = -1.0e9
    C["FM"] = fmv
    sl = np.asarray(SLOPES, dtype=np.float64)

    def btab(D, heads):
        out = np.empty((128, len(heads), D.shape[1]), np.float32)
        for i, h in enumerate(heads):
            out[:, i, :] = np.where(D >= MASKV, -3.0e4, -sl[h] * D.astype(np.float64))
        return out

    C["BA"] = btab(C["D_A"], range(0, 8))
    C["BW"] = btab(C["D_W"], range(8, 16))
    C["BC"] = btab(C["D_C"], range(8, 16))
    C["TRIL"] = (np.arange(128)[:, None] <= np.arange(128)[None, :]).astype(np.float32)
    C["ident"] = np.eye(128, dtype=np.float32)
    return C


def make_in_maps(inp, ncores=8):
    f = lambda a: np.ascontiguousarray(np.asarray(a, dtype=np.float32))
    shared = {
        "att_w_in": f(inp["att_w_in"][0]), "att_sinks": f(inp["att_sinks"][0]).reshape(1, 8),
        "cmp_pe_k": f(np.asarray(inp["cmp_pe_k"][0]).reshape(16, 128).T), "cmp_wk1": f(inp["cmp_wk1"][0]), "cmp_wk2": f(inp["cmp_wk2"][0]),
        "cmp_pe_v": f(np.asarray(inp["cmp_pe_v"][0]).reshape(16, 128).T), "cmp_wv1": f(inp["cmp_wv1"][0]), "cmp_wv2": f(inp["cmp_wv2"][0]),
        "att_w_o": f(inp["att_w_o"][0]),
        "ffn_w_gate": f(inp["ffn_w_gate"][0]), "ffn_w_up": f(inp["ffn_w_up"][0]), "ffn_w_down": f(inp["ffn_w_down"][0]),
        "gmlp_w_in": f(inp["gmlp_w_in"][0]), "gmlp_ln_g": f(inp["gmlp_ln_g"][0]).reshape(1, DM), "gmlp_ln_b": f(inp["gmlp_ln_b"][0]).reshape(1, DM),
        "gmlp_wsT": f(np.transpose(np.asarray(inp["gmlp_w_s"][0]), (2, 0, 1))), "gmlp_bsT": f(np.asarray(inp["gmlp_b_s"][0]).T),
        "gmlp_w_out": f(inp["gmlp_w_out"][0]),
        "moe_w_router": f(inp["moe_w_router"][0]),
        "moe_w_gate": f(inp["moe_w_gate"][0]), "moe_w_up": f(inp["moe_w_up"][0]), "moe_w_down": f(inp["moe_w_down"][0]),
        "ln_g": f(np.asarray(inp["ln_g"]).reshape(4, DM)), "ln_b": f(np.asarray(inp["ln_b"]).reshape(4, DM)),
    }
    shared.update(host_consts())
    x = np.asarray(inp["x"], dtype=np.float32)
    maps = []
    for c in range(ncores):
        xs = np.ascontiguousarray(x[2 * c:2 * c + 2].reshape(NTOK, DM))
        m = dict(shared)
        m["x"] = xs
        m["xT"] = np.ascontiguousarray(xs.T)
        maps.append(m)
    return maps


def kernel(**inputs):
    nc = build_program("moe", False)
    maps = make_in_maps(inputs, 8)
    res = run_bass_kernel_spmd(nc, maps, core_ids=list(range(8)))
    out = np.stack([np.asarray(r["y"]).reshape(2, SEQ, DM) for r in res.results], axis=0)
    return out.reshape(16, SEQ, DM).astype(np.float32)
```
